# Optimizing a Trainium2 kernel written in Bass

```python
import math
import jax, jax.numpy as jnp
from jax import lax
import numpy as np

D_MODEL = 4096
BATCH = 2
SEQ = 4096
DEPTH = 2

GRID_W = 64
CTX_LEN = 256
ATTN_HEADS = 16
ATTN_KV_HEADS = 4
HEAD_DIM = 128
ATTN_GROUP = ATTN_HEADS // ATTN_KV_HEADS
ATTN_DIM = ATTN_HEADS * HEAD_DIM
KV_DIM = ATTN_KV_HEADS * HEAD_DIM
WINDOW = 128
BLOCK = 128
ROPE_THETA = 10000.0
SSM_HEADS = 32
SSM_HEAD_DIM = 64
SSM_DIM = SSM_HEADS * SSM_HEAD_DIM
SSM_GROUPS = 4
SSM_HEADS_PER_GROUP = SSM_HEADS // SSM_GROUPS
SSM_STATE = 128
BC_DIM = SSM_GROUPS * SSM_STATE
CONV_K = 5
CONV_DIM = SSM_DIM + 2 * BC_DIM
CHUNK = 128
N_DIR = 2
D_MIX = ATTN_DIM + SSM_DIM
IN_DIM = ATTN_DIM + SSM_DIM + 2 * KV_DIM + CONV_DIM + N_DIR * SSM_HEADS
CTX_COL0 = ATTN_DIM + SSM_DIM
D_FF = 11008
N_EXPERTS = 8
TOP_K = 2
D_EXPERT = 4096
N_DENSE = (DEPTH + 1) // 2
N_MOE = DEPTH // 2
N_MOD = 6
EPS = 1e-6

kernel_name = "hybrid_swa_ssd_moe_dit_trunk"


def rms_norm(x, g):
    xf = x.astype(jnp.float32)
    xf = xf * lax.rsqrt(jnp.mean(xf * xf, axis=-1, keepdims=True) + EPS)
    return (xf * g.astype(jnp.float32)).astype(x.dtype)


def axial_rope(u, row, col):
    half = HEAD_DIM // 2
    freqs = ROPE_THETA ** (-jnp.arange(0, half, 2, dtype=jnp.float32) / half)

    def rot(v, pos):
        ang = pos.astype(jnp.float32)[:, None] * freqs
        cos = jnp.cos(ang)[:, None, :].astype(v.dtype)
        sin = jnp.sin(ang)[:, None, :].astype(v.dtype)
        v1, v2 = jnp.split(v, 2, axis=-1)
        return jnp.concatenate([v1 * cos - v2 * sin, v2 * cos + v1 * sin], axis=-1)

    return jnp.concatenate([rot(u[..., :half], row), rot(u[..., half:], col)], axis=-1)


def softmax_with_sink(logits, sink):
    full = jnp.concatenate([logits, jnp.broadcast_to(sink, logits.shape[:-1] + (1,))], axis=-1)
    return jax.nn.softmax(full, axis=-1)[..., :-1]


def latent_window_attention(q, k, v, kc, vc, sink):
    b, s = q.shape[:2]
    nb = s // BLOCK
    scale = HEAD_DIM ** -0.5
    qb = q.reshape(b, nb, BLOCK, ATTN_KV_HEADS, ATTN_GROUP, HEAD_DIM)

    def band(u):
        up = jnp.pad(u, ((0, 0), (BLOCK, BLOCK), (0, 0), (0, 0)))
        up = up.reshape(b, nb + 2, BLOCK, ATTN_KV_HEADS, HEAD_DIM)
        return jnp.concatenate([up[:, :-2], up[:, 1:-1], up[:, 2:]], axis=2)

    kb, vb = band(k), band(v)
    s_loc = jnp.einsum('bnqhgd,bnkhd->bnhgqk', qb, kb).astype(jnp.float32) * scale
    blk = jnp.arange(nb)[:, None]
    qpos = blk * BLOCK + jnp.arange(BLOCK)[None, :]
    kpos = (blk - 1) * BLOCK + jnp.arange(3 * BLOCK)[None, :]
    valid = ((jnp.abs(qpos[:, :, None] - kpos[:, None, :]) <= WINDOW)
             & (kpos[:, None, :] >= 0) & (kpos[:, None, :] < s))
    s_loc = jnp.where(valid[None, :, None, None], s_loc, -jnp.inf)
    s_ctx = jnp.einsum('bnqhgd,blhd->bnhgql', qb, kc).astype(jnp.float32) * scale
    sink_b = sink.astype(jnp.float32).reshape(ATTN_KV_HEADS, ATTN_GROUP, 1, 1)
    p = softmax_with_sink(jnp.concatenate([s_loc, s_ctx], axis=-1), sink_b).astype(v.dtype)
    o = (jnp.einsum('bnhgqk,bnkhd->bnqhgd', p[..., :3 * BLOCK], vb)
         + jnp.einsum('bnhgql,blhd->bnqhgd', p[..., 3 * BLOCK:], vc))
    return o.reshape(b, s, ATTN_DIM)


def context_attention(qc, kc, vc, sink):
    b, l = qc.shape[:2]
    qg = qc.reshape(b, l, ATTN_KV_HEADS, ATTN_GROUP, HEAD_DIM)
    sc = jnp.einsum('bqhgd,bkhd->bhgqk', qg, kc).astype(jnp.float32) * HEAD_DIM ** -0.5
    p = softmax_with_sink(sc, sink.astype(jnp.float32).reshape(ATTN_KV_HEADS, ATTN_GROUP, 1, 1)).astype(vc.dtype)
    return jnp.einsum('bhgqk,bkhd->bqhgd', p, vc).reshape(b, l, ATTN_DIM)


def centred_dwconv(u, w, bias):
    out = lax.conv_general_dilated(
        u, w[:, None, :].astype(u.dtype), window_strides=(1,),
        padding=[(CONV_K // 2, CONV_K // 2)], dimension_numbers=('NWC', 'WIO', 'NWC'),
        feature_group_count=u.shape[-1])
    return jax.nn.silu(out + bias.astype(u.dtype))


def ssd_inputs(xbc, dt_raw, conv_w, conv_b, dt_bias):
    u = centred_dwconv(xbc, conv_w, conv_b).astype(jnp.float32)
    b, t = u.shape[:2]
    xs = u[..., :SSM_DIM].reshape(b, t, SSM_GROUPS, SSM_HEADS_PER_GROUP, SSM_HEAD_DIM)
    bm = u[..., SSM_DIM:SSM_DIM + BC_DIM].reshape(b, t, SSM_GROUPS, SSM_STATE)
    cm = u[..., SSM_DIM + BC_DIM:].reshape(b, t, SSM_GROUPS, SSM_STATE)
    dt = jax.nn.softplus(dt_raw.astype(jnp.float32).reshape(b, t, N_DIR, SSM_HEADS)
                         + dt_bias.astype(jnp.float32))
    return xs, bm, cm, dt.reshape(b, t, N_DIR, SSM_GROUPS, SSM_HEADS_PER_GROUP)


def ssd_chunked(x, dt, A, bm, cm, h0):
    b, t = x.shape[:2]
    nc = t // CHUNK
    a = (dt * A).reshape(b, nc, CHUNK, SSM_GROUPS, SSM_HEADS_PER_GROUP)
    xdt = (x * dt[..., None]).reshape(b, nc, CHUNK, SSM_GROUPS, SSM_HEADS_PER_GROUP, SSM_HEAD_DIM)
    bc = bm.reshape(b, nc, CHUNK, SSM_GROUPS, SSM_STATE)
    cc = cm.reshape(b, nc, CHUNK, SSM_GROUPS, SSM_STATE)
    a_cum = jnp.cumsum(a, axis=2)
    a_t = jnp.moveaxis(a_cum, 2, -1)
    seg = a_t[..., :, None] - a_t[..., None, :]
    causal = jnp.tril(jnp.ones((CHUNK, CHUNK), dtype=bool))
    decay_in = jnp.exp(jnp.where(causal, seg, -jnp.inf))
    cb = jnp.einsum('bclgn,bcsgn->bcgls', cc, bc)
    y_diag = jnp.einsum('bcgls,bcgels,bcsgep->bclgep', cb, decay_in, xdt)
    decay_to_end = jnp.exp(a_cum[:, :, -1:] - a_cum)
    states = jnp.einsum('bclgn,bclge,bclgep->bcgepn', bc, decay_to_end, xdt)
    chunk_decay = jnp.exp(a_cum[:, :, -1])

    def step(h, inp):
        dec, st = inp
        return dec[..., None, None] * h + st, h

    h_last, h_prev = lax.scan(step, h0, (jnp.moveaxis(chunk_decay, 1, 0), jnp.moveaxis(states, 1, 0)))
    h_prev = jnp.moveaxis(h_prev, 0, 1)
    y_off = jnp.einsum('bclgn,bcgepn,bclge->bclgep', cc, h_prev, jnp.exp(a_cum))
    return (y_diag + y_off).reshape(b, t, SSM_GROUPS, SSM_HEADS_PER_GROUP, SSM_HEAD_DIM), h_last


def ssd_final_state(x, dt, A, bm):
    a_cum = jnp.cumsum(dt * A, axis=1)
    decay_to_end = jnp.exp(a_cum[:, -1:] - a_cum)
    return jnp.einsum('btgn,btge,btgep->bgepn', bm, decay_to_end, x * dt[..., None])


def hybrid_mixer(h, hc, w_in, attn_sink, attn_norm, conv_w, conv_b, dt_bias, a_log, d_skip,
                 ssm_norm, w_out, last):
    b, s, _ = h.shape
    l = hc.shape[1]
    rows = s // GRID_W
    row = jnp.repeat(jnp.arange(rows, dtype=jnp.int32), GRID_W)
    col = jnp.tile(jnp.arange(GRID_W, dtype=jnp.int32), rows)
    off_kv = [KV_DIM, 2 * KV_DIM, 2 * KV_DIM + CONV_DIM]
    off_all = [ATTN_DIM, ATTN_DIM + SSM_DIM] + [CTX_COL0 + o for o in off_kv]

    q, z, k, v, xbc, dt_raw = jnp.split(h @ w_in, off_all, axis=-1)
    if last:
        kc, vc, xbcc, dtc_raw = jnp.split(hc @ w_in[:, CTX_COL0:], off_kv, axis=-1)
    else:
        qc, zc, kc, vc, xbcc, dtc_raw = jnp.split(hc @ w_in, off_all, axis=-1)
    kc = kc.reshape(b, l, ATTN_KV_HEADS, HEAD_DIM)
    vc = vc.reshape(b, l, ATTN_KV_HEADS, HEAD_DIM)

    q = axial_rope(q.reshape(b, s, ATTN_HEADS, HEAD_DIM), row, col)
    k = axial_rope(k.reshape(b, s, ATTN_KV_HEADS, HEAD_DIM), row, col)
    v = v.reshape(b, s, ATTN_KV_HEADS, HEAD_DIM)
    attn = rms_norm(latent_window_attention(q, k, v, kc, vc, attn_sink), attn_norm)

    A = -jnp.exp(a_log.astype(jnp.float32)).reshape(N_DIR, SSM_GROUPS, SSM_HEADS_PER_GROUP)
    d_vec = d_skip.astype(jnp.float32).reshape(SSM_GROUPS, SSM_HEADS_PER_GROUP, 1)
    xs, bm, cm, dt = ssd_inputs(xbc, dt_raw, conv_w, conv_b, dt_bias)
    xsc, bmc, cmc, dtc = ssd_inputs(xbcc, dtc_raw, conv_w, conv_b, dt_bias)
    flip = lambda u: jnp.flip(u, axis=1)

    def ssd_out(y, xs_, z_):
        y = (y + d_vec * xs_).reshape(z_.shape).astype(z_.dtype)
        return rms_norm(y * jax.nn.silu(z_), ssm_norm)

    if last:
        hc_f = ssd_final_state(xsc, dtc[:, :, 0], A[0], bmc)
        hc_b = ssd_final_state(flip(xsc), flip(dtc[:, :, 1]), A[1], flip(bmc))
    else:
        zero = jnp.zeros((b, SSM_GROUPS, SSM_HEADS_PER_GROUP, SSM_HEAD_DIM, SSM_STATE), jnp.float32)
        yc_f, hc_f = ssd_chunked(xsc, dtc[:, :, 0], A[0], bmc, cmc, zero)
        yc_b, hc_b = ssd_chunked(flip(xsc), flip(dtc[:, :, 1]), A[1], flip(bmc), flip(cmc), zero)
    y_f, _ = ssd_chunked(xs, dt[:, :, 0], A[0], bm, cm, hc_f)
    y_b, _ = ssd_chunked(flip(xs), flip(dt[:, :, 1]), A[1], flip(bm), flip(cm), hc_b)
    ssm = ssd_out(y_f + flip(y_b), xs, z)
    out = jnp.concatenate([attn, ssm], axis=-1) @ w_out
    if last:
        return out, None
    attn_c = rms_norm(context_attention(qc.reshape(b, l, ATTN_HEADS, HEAD_DIM), kc, vc, attn_sink), attn_norm)
    ssm_c = ssd_out(yc_f + flip(yc_b), xsc, zc)
    out_c = jnp.concatenate([attn_c, ssm_c], axis=-1) @ w_out
    return out, out_c


def swiglu(h, wg, wu, wd):
    return (jax.nn.silu(h @ wg) * (h @ wu)) @ wd


def moe_ffn(h, router, wg, wu, wd):
    tok = h.reshape(-1, D_MODEL)
    logits = (tok @ router).astype(jnp.float32)
    top_val, top_idx = lax.top_k(logits, TOP_K)
    gates = jax.nn.softmax(top_val, axis=-1)
    combine = jnp.sum(jax.nn.one_hot(top_idx, N_EXPERTS, dtype=jnp.float32) * gates[..., None], axis=1)
    out = jnp.zeros_like(tok)
    for e in range(N_EXPERTS):
        out = out + combine[:, e:e + 1].astype(tok.dtype) * swiglu(tok, wg[e], wu[e], wd[e])
    return out.reshape(h.shape)


def setup_inputs(seed: int = 0) -> dict:
    key = jax.random.key(seed)
    ks = jax.random.split(key, 32)
    f32 = jnp.float32
    nrm = lambda k, shape, scale: jax.random.normal(k, shape, f32) * scale
    gain = lambda k, shape: 1.0 + 0.1 * jax.random.normal(k, shape, f32)
    dt0 = jnp.exp(jax.random.uniform(ks[15], (DEPTH, N_DIR, SSM_HEADS), f32, math.log(1e-3), math.log(1e-1)))
    return {
        "x": nrm(ks[0], (BATCH, SEQ, D_MODEL), 1.0),
        "c": nrm(ks[1], (BATCH, D_MODEL), 1.0),
        "ctx": nrm(ks[2], (BATCH, CTX_LEN, D_MODEL), 1.0),
        "c_ctx": nrm(ks[3], (D_MODEL,), 1.0),
        "ada_w": nrm(ks[4], (DEPTH, D_MODEL, N_MOD * D_MODEL), 0.5 * D_MODEL ** -0.5),
        "ada_b": nrm(ks[5], (DEPTH, N_MOD * D_MODEL), 0.02),
        "norm_mix_pre": gain(ks[6], (DEPTH, D_MODEL)),
        "norm_mix_post": gain(ks[7], (DEPTH, D_MODEL)),
        "norm_ffn_pre": gain(ks[8], (DEPTH, D_MODEL)),
        "norm_ffn_post": gain(ks[9], (DEPTH, D_MODEL)),
        "w_in": nrm(ks[10], (DEPTH, D_MODEL, IN_DIM), D_MODEL ** -0.5),
        "attn_sink": nrm(ks[11], (DEPTH, ATTN_HEADS), 0.5),
        "attn_norm": gain(ks[12], (DEPTH, ATTN_DIM)),
        "conv_w": nrm(ks[13], (DEPTH, CONV_K, CONV_DIM), CONV_K ** -0.5),
        "conv_b": nrm(ks[14], (DEPTH, CONV_DIM), 0.02),
        "dt_bias": dt0 + jnp.log(-jnp.expm1(-dt0)),
        "a_log": jnp.log(jax.random.uniform(ks[16], (DEPTH, N_DIR, SSM_HEADS), f32, 1.0, 16.0)),
        "d_skip": gain(ks[17], (DEPTH, SSM_HEADS)),
        "ssm_norm": gain(ks[18], (DEPTH, SSM_DIM)),
        "w_out": nrm(ks[19], (DEPTH, D_MIX, D_MODEL), D_MIX ** -0.5),
        "ffn_w_gate": nrm(ks[20], (N_DENSE, D_MODEL, D_FF), D_MODEL ** -0.5),
        "ffn_w_up": nrm(ks[21], (N_DENSE, D_MODEL, D_FF), D_MODEL ** -0.5),
        "ffn_w_down": nrm(ks[22], (N_DENSE, D_FF, D_MODEL), D_FF ** -0.5),
        "moe_router": nrm(ks[23], (N_MOE, D_MODEL, N_EXPERTS), D_MODEL ** -0.5),
        "moe_w_gate": nrm(ks[24], (N_MOE, N_EXPERTS, D_MODEL, D_EXPERT), D_MODEL ** -0.5),
        "moe_w_up": nrm(ks[25], (N_MOE, N_EXPERTS, D_MODEL, D_EXPERT), D_MODEL ** -0.5),
        "moe_w_down": nrm(ks[26], (N_MOE, N_EXPERTS, D_EXPERT, D_MODEL), D_EXPERT ** -0.5),
    }


def reference(x, c, ctx, c_ctx, ada_w, ada_b, norm_mix_pre, norm_mix_post, norm_ffn_pre, norm_ffn_post,
              w_in, attn_sink, attn_norm, conv_w, conv_b, dt_bias, a_log, d_skip, ssm_norm, w_out,
              ffn_w_gate, ffn_w_up, ffn_w_down, moe_router, moe_w_gate, moe_w_up, moe_w_down):
    c_act = jax.nn.silu(c)
    cc_act = jax.nn.silu(c_ctx)
    xc = ctx
    l = ctx.shape[1]
    for i in range(DEPTH):
        last = i == DEPTH - 1
        mod = (c_act @ ada_w[i] + ada_b[i])[:, None, :]
        sh_m, sc_m, g_m, sh_f, sc_f, g_f = jnp.split(mod, N_MOD, axis=-1)
        n_ctx_mod = 2 if last else N_MOD
        modc = cc_act @ ada_w[i][:, :n_ctx_mod * D_MODEL] + ada_b[i][:n_ctx_mod * D_MODEL]
        mc = jnp.split(modc, n_ctx_mod)
        h = rms_norm(x, norm_mix_pre[i]) * (1 + sc_m) + sh_m
        hc = rms_norm(xc, norm_mix_pre[i]) * (1 + mc[1]) + mc[0]
        y, yc = hybrid_mixer(h, hc, w_in[i], attn_sink[i], attn_norm[i], conv_w[i], conv_b[i],
                             dt_bias[i], a_log[i], d_skip[i], ssm_norm[i], w_out[i], last)
        x = x + g_m * rms_norm(y, norm_mix_post[i])
        hf = rms_norm(x, norm_ffn_pre[i]) * (1 + sc_f) + sh_f
        if not last:
            xc = xc + mc[2] * rms_norm(yc, norm_mix_post[i])
            hfc = rms_norm(xc, norm_ffn_pre[i]) * (1 + mc[4]) + mc[3]
            hf = jnp.concatenate([hfc, hf], axis=1)
        if i % 2 == 0:
            j = i // 2
            f = swiglu(hf, ffn_w_gate[j], ffn_w_up[j], ffn_w_down[j])
        else:
            j = i // 2
            f = moe_ffn(hf, moe_router[j], moe_w_gate[j], moe_w_up[j], moe_w_down[j])
        if last:
            x = x + g_f * rms_norm(f, norm_ffn_post[i])
        else:
            x = x + g_f * rms_norm(f[:, l:], norm_ffn_post[i])
            xc = xc + mc[5] * rms_norm(f[:, :l], norm_ffn_post[i])
    return x
```

```python
import contextlib
import os
import numpy as np
import ml_dtypes
import concourse.bass as bass
import concourse.mybir as mybir
from concourse.bass_utils import run_bass_kernel_spmd

F32 = mybir.dt.float32
BF16 = mybir.dt.bfloat16
AF = mybir.ActivationFunctionType
ALU = mybir.AluOpType
AX = mybir.AxisListType

ENGS = ("pe", "act", "dve", "pool", "sp")
N_DMA_SEMS = 24


class _State:
    __slots__ = ("last_w", "readers")

    def __init__(self):
        self.last_w = None
        self.readers = {}


class Buf:
    def __init__(self, fw, name, t, kind):
        self.fw = fw
        self.name = name
        self.t = t
        self.kind = kind
        self.whole = _State()
        self.parts = {}

    def __getitem__(self, idx):
        return self.t[idx]


class FW:
    def __init__(self, name="k"):
        self.nc = bass.Bass("TRN2", target_bir_lowering=False)
        self.stack = contextlib.ExitStack()
        self.ops = {e: [] for e in ENGS}
        self.cnt = {e: 0 for e in ENGS}
        self.known = {e: {} for e in ENGS}
        self.snap = {e: [None] for e in ENGS}
        self.sems = {}
        for e in ("pe", "act", "dve", "pool"):
            self.sems[e] = self.stack.enter_context(self.nc.semaphore("s_" + e))
        self.dma_sems = [self.stack.enter_context(self.nc.semaphore("s_dma%d" % i)) for i in range(N_DMA_SEMS)]
        self.dma_val = [0] * N_DMA_SEMS
        self.dma_rr = 0
        self.out_events = []
        self.nbuf = 0
        self.cur_stack = self.stack

    def sbuf(self, shape, dtype, name=None):
        self.nbuf += 1
        name = (name or "sb") + "_%d" % self.nbuf
        t = self.cur_stack.enter_context(self.nc.sbuf_tensor(name, list(shape), dtype))
        return Buf(self, name, t, "sbuf")

    @contextlib.contextmanager
    def phase(self):
        prev = self.cur_stack
        st = contextlib.ExitStack()
        self.cur_stack = st
        try:
            yield
        finally:
            self.barrier()
            st.close()
            self.cur_stack = prev

    def barrier(self):
        evs = [("eng", f, self.cnt[f]) for f in ("pe", "act", "dve", "pool") if self.cnt[f] > 0]
        evs += [("dma", i, self.dma_val[i]) for i in range(N_DMA_SEMS) if self.dma_val[i] > 0]
        for e in ENGS:
            waits = self._waits(e, evs)
            if waits:
                self.ops[e].append((waits, None, None))

    def psum(self, shape, dtype=F32, name=None):
        self.nbuf += 1
        name = (name or "ps") + "_%d" % self.nbuf
        t = self.cur_stack.enter_context(self.nc.psum_tensor(name, list(shape), dtype))
        return Buf(self, name, t, "psum")

    def dram(self, name, shape, dtype, kind):
        t = self.nc.dram_tensor(name, list(shape), dtype, kind=kind)
        return Buf(self, name, t.ap(), "dram_" + kind)

    @staticmethod
    def _norm(acc):
        if isinstance(acc, Buf):
            return acc, None
        if acc[0].kind == "psum":
            return acc[0], None
        return acc

    def _collect(self, reads, writes):
        deps = []
        for acc in reads:
            b, p = self._norm(acc)
            if b.kind == "dram_ExternalInput":
                continue
            if b.whole.last_w is not None:
                deps.append(b.whole.last_w)
            if p is None:
                for st in b.parts.values():
                    if st.last_w is not None:
                        deps.append(st.last_w)
            else:
                st = b.parts.get(p)
                if st is not None and st.last_w is not None:
                    deps.append(st.last_w)
        for acc in writes:
            b, p = self._norm(acc)
            if b.whole.last_w is not None:
                deps.append(b.whole.last_w)
            deps.extend(b.whole.readers.values())
            if p is None:
                for st in b.parts.values():
                    if st.last_w is not None:
                        deps.append(st.last_w)
                    deps.extend(st.readers.values())
            else:
                st = b.parts.get(p)
                if st is not None:
                    if st.last_w is not None:
                        deps.append(st.last_w)
                    deps.extend(st.readers.values())
        return deps

    def _record(self, reads, writes, ev):
        key = ev[1] if ev[0] == "eng" else ev
        for acc in reads:
            b, p = self._norm(acc)
            if b.kind == "dram_ExternalInput":
                continue
            st = b.whole if p is None else b.parts.setdefault(p, _State())
            st.readers[key] = ev
        for acc in writes:
            b, p = self._norm(acc)
            if p is None:
                b.parts = {}
                b.whole.last_w = ev
                b.whole.readers = {}
            else:
                st = b.parts.setdefault(p, _State())
                st.last_w = ev
                st.readers = {}

    def _waits(self, e, deps):
        kn = self.known[e]
        need = {}
        for ev in deps:
            if ev[0] == "eng":
                _, f, n = ev
                if f == e and e == "pe":
                    continue
                k = ("eng", f)
            else:
                _, s, n = ev
                k = ("dma", s)
            if kn.get(k, 0) >= n:
                continue
            if need.get(k, 0) < n:
                need[k] = n
        waits = []
        for k, n in need.items():
            if kn.get(k, 0) >= n:
                continue
            kn[k] = n
            if k[0] == "eng":
                waits.append((self.sems[k[1]], n))
                sn = self.snap[k[1]][n]
                if sn:
                    for kk, vv in sn.items():
                        if kn.get(kk, 0) < vv:
                            kn[kk] = vv
            else:
                waits.append((self.dma_sems[k[1]], n))
        return waits

    def op(self, e, fn, reads=(), writes=()):
        deps = self._collect(reads, writes)
        waits = self._waits(e, deps)
        self.cnt[e] += 1
        n = self.cnt[e]
        if e == "pe":
            self.known[e][("eng", e)] = n
        self.snap[e].append(dict(self.known[e]))
        self.ops[e].append((waits, fn, (self.sems[e], 1)))
        ev = ("eng", e, n)
        self._record(reads, writes, ev)
        return ev

    def dma(self, q, out_buf, out_ap, in_buf, in_ap, out_part=None, in_part=None, **kw):
        reads = [(in_buf, in_part)]
        writes = [(out_buf, out_part)]
        deps = self._collect(reads, writes)
        s = self.dma_rr
        self.dma_rr = (self.dma_rr + 1) % N_DMA_SEMS
        if self.dma_val[s] > 0:
            deps.append(("dma", s, self.dma_val[s]))
        waits = self._waits(q, deps)
        self.dma_val[s] += 16
        ev = ("dma", s, self.dma_val[s])

        def fn(eng, out_ap=out_ap, in_ap=in_ap, kw=kw):
            return eng.dma_start(out=out_ap, in_=in_ap, **kw)

        self.ops[q].append((waits, fn, (self.dma_sems[s], 16)))
        self._record(reads, writes, ev)
        if out_buf.kind == "dram_ExternalOutput":
            self.out_events.append(ev)
        return ev

    def mm(self, out, out_ap, lhsT, lhsT_ap, rhs, rhs_ap, start=True, stop=True, out_part=None,
           lhsT_part=None, rhs_part=None):
        return self.op("pe", lambda eng: eng.matmul(out_ap, lhsT_ap, rhs_ap, start=start, stop=stop),
                       reads=[(lhsT, lhsT_part), (rhs, rhs_part)], writes=[(out, out_part)])

    def tr(self, out, out_ap, in_, in_ap, ident, out_part=None, in_part=None):
        return self.op("pe", lambda eng: eng.transpose(out_ap, in_ap, ident.t[:]),
                       reads=[(in_, in_part), ident], writes=[(out, out_part)])

    @staticmethod
    def _acc(o):
        return (o[0], o[2] if len(o) > 2 else None)

    def _rw(self, outs, ins):
        reads = [self._acc(o) for o in ins if isinstance(o, tuple)]
        writes = [self._acc(o) for o in outs]
        return reads, writes

    @staticmethod
    def _ap(o):
        return o[1] if isinstance(o, tuple) else o

    def activation(self, out, in_, func, scale=1.0, bias=0.0, eng="act"):
        r, w = self._rw([out], [in_, scale, bias])
        return self.op(eng, lambda e: e.activation(out=out[1], in_=in_[1], func=func, scale=self._ap(scale),
                                                   bias=self._ap(bias)), r, w)

    def tt(self, eng, out, in0, in1, op):
        r, w = self._rw([out], [in0, in1])
        return self.op(eng, lambda e: e.tensor_tensor(out=out[1], in0=in0[1], in1=in1[1], op=op), r, w)

    def ts(self, eng, out, in0, s1, op0, s2=None, op1=None):
        r, w = self._rw([out], [in0, s1, s2])
        if op1 is None:
            return self.op(eng, lambda e: e.tensor_scalar(out=out[1], in0=in0[1], scalar1=self._ap(s1), scalar2=None,
                                                          op0=op0), r, w)
        return self.op(eng, lambda e: e.tensor_scalar(out=out[1], in0=in0[1], scalar1=self._ap(s1),
                                                      scalar2=self._ap(s2), op0=op0, op1=op1), r, w)

    def stt(self, out, in0, scalar, in1, op0, op1):
        r, w = self._rw([out], [in0, scalar, in1])
        return self.op("dve", lambda e: e.scalar_tensor_tensor(out=out[1], in0=in0[1], scalar=self._ap(scalar),
                                                               in1=in1[1], op0=op0, op1=op1), r, w)

    def copy(self, eng, out, in_):
        r, w = self._rw([out], [in_])
        if eng == "act":
            return self.op(eng, lambda e: e.copy(out=out[1], in_=in_[1]), r, w)
        return self.op(eng, lambda e: e.tensor_copy(out=out[1], in_=in_[1]), r, w)

    def memset(self, eng, out, val):
        r, w = self._rw([out], [])
        return self.op(eng, lambda e: e.memset(out[1], val), r, w)

    def rsum(self, eng, out, in_):
        r, w = self._rw([out], [in_])
        return self.op(eng, lambda e: e.reduce_sum(out=out[1], in_=in_[1], axis=AX.X), r, w)

    def recip(self, out, in_):
        r, w = self._rw([out], [in_])
        return self.op("dve", lambda e: e.reciprocal(out=out[1], in_=in_[1]), r, w)

    def finish(self):
        nc = self.nc
        deps = list(self.out_events)
        waits = self._waits("sp", deps)
        self.ops["sp"].append((waits, None, None))

        def emit(eng, lst):
            for waits, fn, inc in lst:
                for sem, val in waits:
                    eng.wait_ge(sem, val)
                if fn is not None:
                    ins = fn(eng)
                    ins.then_inc(inc[0], inc[1])

        with nc.Block() as block:
            @block.tensor
            def _(eng):
                emit(eng, self.ops["pe"])

            @block.scalar
            def _(eng):
                emit(eng, self.ops["act"])

            @block.vector
            def _(eng):
                emit(eng, self.ops["dve"])

            @block.gpsimd
            def _(eng):
                emit(eng, self.ops["pool"])

            @block.sync
            def _(eng):
                emit(eng, self.ops["sp"])
        self.stack.close()
        return nc


MOD_COLS = 3072


def build_mod(nl=2, ncols=MOD_COLS, kdim=4096):
    fw = FW()
    KC = kdim // 128
    cT_d = fw.dram("cT", [128, KC, 3], F32, "ExternalInput")
    aw_d = fw.dram("aw", [nl, kdim, ncols], F32, "ExternalInput")
    ab_d = fw.dram("ab", [3, nl * ncols], F32, "ExternalInput")
    out_d = fw.dram("mod", [3, nl * ncols], F32, "ExternalOutput")
    cT = fw.sbuf([128, KC, 3], F32, "cT")
    cS = fw.sbuf([128, KC, 3], F32, "cS")
    ab = fw.sbuf([3, nl * ncols], F32, "ab")
    res = fw.sbuf([3, nl * ncols], F32, "res")
    wb = [fw.sbuf([128, KC, 512], F32, "wb") for _ in range(2)]
    ps = [fw.psum([3, 512], F32, "ps") for _ in range(2)]
    fw.dma("sp", cT, cT[:], cT_d, cT_d[:])
    fw.dma("sp", ab, ab[:], ab_d, ab_d[:])
    fw.op("act", lambda e: e.activation(out=cS[:], in_=cT[:], func=AF.Silu), reads=[cT], writes=[cS])
    nchunk = ncols // 512
    it = 0
    for l in range(nl):
        for j in range(nchunk):
            w = wb[it % 2]
            p = ps[it % 2]
            src = aw_d[l, :, j * 512:(j + 1) * 512].rearrange("(kc p) n -> p kc n", p=128)
            half = KC // 2
            fw.dma("sp", w, w[:, 0:half, :], aw_d, src[:, 0:half, :], out_part=0)
            fw.dma("act", w, w[:, half:KC, :], aw_d, src[:, half:KC, :], out_part=1)
            for kc in range(KC):
                fw.mm(p, p[:, :], cS, cS[:, kc, :], w, w[:, kc, :], start=(kc == 0), stop=(kc == KC - 1),
                      rhs_part=(0 if kc < half else 1))
            o = l * ncols + j * 512
            fw.op("dve", lambda e, p=p, o=o: e.tensor_tensor(out=res[:, o:o + 512], in0=p[:, :], in1=ab[:, o:o + 512],
                                                             op=ALU.add),
                  reads=[p, ab], writes=[(res, o)])
            it += 1
    fw.dma("sp", out_d, out_d[:], res, res[:])
    return fw.finish()


def run_mod(c, c_ctx, ada_w, ada_b):
    nl, kdim, ntot = ada_w.shape
    cvec = np.concatenate([c, c_ctx[None, :]], axis=0).astype(np.float32)
    KC = kdim // 128
    cT = np.ascontiguousarray(cvec.T.reshape(KC, 128, 3).transpose(1, 0, 2))
    ncols = ntot // 8
    nc = build_mod(nl, ncols, kdim)
    in_maps = []
    for k in range(8):
        sl = slice(k * ncols, (k + 1) * ncols)
        ab = np.concatenate([np.broadcast_to(ada_b[l, sl][None, :], (3, ncols)) for l in range(nl)], axis=1)
        in_maps.append({"cT": cT, "aw": np.ascontiguousarray(ada_w[:, :, sl]), "ab": np.ascontiguousarray(ab)})
    res = run_bass_kernel_spmd(nc, in_maps, core_ids=list(range(8)))
    mod = np.zeros((nl, 3, ntot), np.float32)
    for k in range(8):
        r = res.results[k]["mod"]
        for l in range(nl):
            mod[l, :, k * ncols:(k + 1) * ncols] = r[:, l * ncols:(l + 1) * ncols]
    return mod


class HTMaker:
    def __init__(self, fw, D, ident, banks, segs=None, eps=1e-6):
        self.fw, self.D, self.KC = fw, D, D // 128
        self.ident = ident
        self.banks = banks
        self.segs = segs or [(0, D)]
        self.eps = eps
        self.xt = [fw.sbuf([128, D], F32, "xt") for _ in range(2)]
        self.junk = fw.sbuf([128, D], BF16, "junk")
        self.ss = [fw.sbuf([128, 2 * len(self.segs)], F32, "ss") for _ in range(2)]
        self.n = 0
        self.nb = 0
        self.xb = [fw.sbuf([128, D], BF16, "xb") for _ in range(2)]
        self.pb = [fw.psum([128, 512], BF16, "pbank") for _ in range(2)]
        self.identb = fw.sbuf([128, 128], BF16, "identb")
        fw.copy("dve", (self.identb, self.identb[:]), (ident, ident.t[:]))

    def make(self, src_buf, src_ap, A, B, aidx, hT, hT_ap3, nrows=128, keep_x=None):
        fw = self.fw
        xt = self.xt[self.n % 2]
        ss = self.ss[self.n % 2]
        self.n += 1
        q = "sp" if self.n % 2 else "act"
        if nrows < 128:
            fw.memset("pool", (xt, xt[:]), 0.0)
        fw.dma(q, xt, xt[0:nrows, :], src_buf, src_ap)
        import os
        HD = int(os.environ.get('HTDBG', '9'))
        if HD < 1:
            return xt
        fw.activation((self.junk, self.junk[:]), (xt, xt[:]), AF.Square)
        ns = len(self.segs)
        if HD < 2:
            return xt
        for i, (a, b) in enumerate(self.segs):
            fw.rsum("dve", (ss, ss[:, i:i + 1]), (self.junk, self.junk[:, a:b]))
            fw.activation((ss, ss[:, ns + i:ns + i + 1]), (ss, ss[:, i:i + 1]), AF.Sqrt, scale=1.0 / (b - a), bias=self.eps)
            fw.recip((ss, ss[:, ns + i:ns + i + 1]), (ss, ss[:, ns + i:ns + i + 1]))
            xb = self.xb[self.n % 2]
            fw.ts("dve", (xb, xb[:, a:b]), (xt, xt[:, a:b]), (ss, ss[:, ns + i:ns + i + 1]), ALU.mult)
        if HD < 3:
            return xt
        for kc in range(self.KC):
            j = kc % 4
            if j == 0:
                bank = self.pb[self.nb % 2]
                self.nb += 1
            fw.tr(bank, bank[:, j * 128:(j + 1) * 128], xb, xb[:, kc * 128:(kc + 1) * 128], self.identb)
            if j == 3 and HD >= 4:
                for jj in range(4):
                    k2 = kc - 3 + jj
                    EV = os.environ.get('EVDBG', '0')
                    if EV == '1':
                        fw.ts("dve", (hT, hT_ap3[:, k2, :], k2), (xt, xt[:, 0:128]),
                              (A, A[:, aidx, k2:k2 + 1]), ALU.mult, (B, B[:, aidx, k2:k2 + 1]), ALU.add)
                    elif EV == '2':
                        fw.copy("dve", (hT, hT_ap3[:, k2, :], k2), (bank, bank[:, jj * 128:(jj + 1) * 128]))
                    elif EV == '5':
                        fw.copy("act", (hT, hT_ap3[:, k2, :], k2), (bank, bank[:, jj * 128:(jj + 1) * 128]))
                    elif EV == '7':
                        fw.copy("act", (hT, hT_ap3[:, k2, :], k2), (bank, bank[:, jj * 128:(jj + 1) * 128]))
                        fw.ts("dve", (hT, hT_ap3[:, k2, :], k2), (hT, hT_ap3[:, k2, :], k2),
                              (A, A[:, aidx, k2:k2 + 1]), ALU.mult, (B, B[:, aidx, k2:k2 + 1]), ALU.add)
                    elif EV == '6':
                        fw.copy("dve", (self.junk, self.junk[:, 0:128]), (bank, bank[:, jj * 128:(jj + 1) * 128]))
                    elif EV == '3':
                        fw.ts("dve", (hT, hT_ap3[:, k2, :], k2), (bank, bank[:, jj * 128:(jj + 1) * 128], jj),
                              (A, A[:, aidx, k2:k2 + 1]), ALU.mult)
                    else:
                        fw.ts("dve", (hT, hT_ap3[:, k2, :], k2), (bank, bank[:, jj * 128:(jj + 1) * 128]),
                              (A, A[:, aidx, k2:k2 + 1]), ALU.mult, (B, B[:, aidx, k2:k2 + 1]), ALU.add)
        return xt


def load_weight_bf16(fw, wsb, w_d, ncol, KC=32, step=8):
    src = w_d[:, :].rearrange("(kc p) n -> p kc n", p=128)
    for k0 in range(0, KC, step):
        fw.dma("pool", wsb, wsb[:, k0:k0 + step, 0:ncol], w_d, src[:, k0:k0 + step, :], out_part=k0 // step)


NA = 1408
NB = 1296


def build_mixer(S, LC=256, D=4096, do_ctx_q=True, stop=9, debug=False):
    fw = FW()
    T = LC + S
    NT, NTC = T // 128, LC // 128
    NL = S // 128
    KC = D // 128
    xin_d = fw.dram("xin", [T, D], F32, "ExternalInput")
    g_d = fw.dram("gT", [128, 2, KC], F32, "ExternalInput")
    sc_d = fw.dram("scT", [128, 2, KC], F32, "ExternalInput")
    sh_d = fw.dram("shT", [128, 2, KC], F32, "ExternalInput")
    wA_d = fw.dram("wA", [D, NA], F32, "ExternalInput")
    wB_d = fw.dram("wB", [D, NB], F32, "ExternalInput")
    cos_d = fw.dram("cosT", [128, T], F32, "ExternalInput")
    sin_d = fw.dram("sinT", [128, T], F32, "ExternalInput")
    cw_d = fw.dram("convw", [128, 6, 5], F32, "ExternalInput")
    cb_d = fw.dram("convb", [128, 6], F32, "ExternalInput")
    sm_d = fw.dram("small", [128, 64], F32, "ExternalInput")
    cst_d = fw.dram("cst", [128, 6, 128], F32, "ExternalInput")
    attn_d = fw.dram("attn", [T, 512], F32, "ExternalOutput")
    yg_d = fw.dram("yg", [T, 512], F32, "ExternalOutput")
    xbc_s = fw.dram("xbc_s", [768, T], F32, "Internal")
    sz_s = fw.dram("sz_s", [T, 512], F32, "Internal")
    ya_s = fw.dram("ya_s", [T, 512], F32, "Internal")

    cst = fw.sbuf([128, 6, 128], F32, "cst")
    cstb = fw.sbuf([128, 2, 128], BF16, "cstb")
    ident = Buf(fw, "ident", None, "sbuf")
    fw.dma("sp", cst, cst[:], cst_d, cst_d[:])
    ident = cst

    class _Id:
        pass
    idb = Buf(fw, "identv", cst.t[:, 0, :], "sbuf")
    idb.whole = cst.whole
    idb.parts = cst.parts
    fw.copy("dve", (cstb, cstb[:, 0, :]), (cst, cst[:, 2, :]))
    fw.copy("dve", (cstb, cstb[:, 1, :]), (cst, cst[:, 1, :]))
    gT = fw.sbuf([128, 2, KC], F32, "gT")
    Am = fw.sbuf([128, 2, KC], F32, "Am")
    Bm = fw.sbuf([128, 2, KC], F32, "Bm")
    fw.dma("sp", gT, gT[:], g_d, g_d[:])
    fw.dma("sp", Am, Am[:], sc_d, sc_d[:])
    fw.dma("sp", Bm, Bm[:], sh_d, sh_d[:])
    fw.stt((Am, Am[:]), (Am, Am[:]), 1.0, (gT, gT[:]), ALU.add, ALU.mult)
    cw = fw.sbuf([128, 6, 5], F32, "cw")
    cb = fw.sbuf([128, 6], F32, "cb")
    sm = fw.sbuf([128, 64], F32, "sm")
    fw.dma("sp", cw, cw[:], cw_d, cw_d[:])
    fw.dma("sp", cb, cb[:], cb_d, cb_d[:])
    fw.dma("sp", sm, sm[:], sm_d, sm_d[:])
    fw.activation((sm, sm[:, 16:32]), (sm, sm[:, 16:32]), AF.Exp)
    fw.ts("dve", (sm, sm[:, 16:32]), (sm, sm[:, 16:32]), -1.0, ALU.mult)
    fw.activation((sm, sm[:, 40:44]), (sm, sm[:, 40:44]), AF.Exp)

    banks = [fw.psum([128, 512], F32, "bank") for _ in range(6)]
    qT_s = fw.dram("qT_s", [128, 4, T], BF16, "Internal")
    kT = fw.sbuf([128, T], BF16, "kT")
    vA = fw.sbuf([128, NT, 132], BF16, "vA")
    dtA = fw.sbuf([128, NT, 16], F32, "dtA")
    aA = fw.sbuf([128, NT, 16], F32, "aA")
    fw.memset("pool", (vA, vA[:]), 1.0)
    ph1 = fw.phase()
    ph1.__enter__()
    wsb = fw.sbuf([128, KC, NA], BF16, "wsb")
    hTb = [fw.sbuf([128, KC, 128], BF16, "hT") for _ in range(2)]
    qtb = [fw.sbuf([128, 4, 128], BF16, "qtb") for _ in range(2)]
    htm = HTMaker(fw, D, idb, banks[0:2])
    cs = [fw.sbuf([128, 2, 128], F32, "cs") for _ in range(2)]
    t1 = fw.sbuf([128, 512], F32, "t1")
    t2 = fw.sbuf([128, 512], F32, "t2")

    def row_ap(i):
        return xin_d[i * 128:(i + 1) * 128, :]

    if stop >= 0:
        load_weight_bf16(fw, wsb, wA_d, NA)
    for i in range(NT if stop >= 1 else 0):
        hT = hTb[i % 2]
        htm.make(xin_d, row_ap(i), Am, Bm, 0 if i < NTC else 1, hT, hT[:])
        import os
        DBG = int(os.environ.get('MIXDBG', '9'))
        if DBG < 1:
            continue
        if debug and i == 0:
            d_hT = fw.dram("d_hT", [128, KC, 128], BF16, "ExternalOutput")
            fw.dma("sp", d_hT, d_hT[:], hT, hT[:])
        c = cs[i % 2]
        fw.dma("sp", c, c[:, 0, :], cos_d, cos_d[:, i * 128:(i + 1) * 128], out_part=0)
        fw.dma("sp", c, c[:, 1, :], sin_d, sin_d[:, i * 128:(i + 1) * 128], out_part=1)
        bq, bqb, bk, bv = banks[2], banks[3], banks[4], banks[5]
        for grp in range(10):
            bank, col = (bq, grp) if grp < 4 else ((bqb, grp - 4) if grp < 8 else (bk, grp - 8))
            for kc in range(KC):
                fw.mm(bank, bank[:, col * 128:(col + 1) * 128], wsb, wsb[:, kc, grp * 128:(grp + 1) * 128],
                      hT, hT[:, kc, :], start=(kc == 0), stop=(kc == KC - 1), out_part=col, lhsT_part=kc // 8)
        for kc in range(KC):
            fw.mm(bv, bv[:, 0:128], hT, hT[:, kc, :], wsb, wsb[:, kc, 1280:1408], start=(kc == 0), stop=(kc == KC - 1),
                  out_part=0, rhs_part=kc // 8)
        if DBG < 2:
            continue
        cosb = c[:, 0, :].unsqueeze(1).broadcast_to([128, 4, 128])
        sinb = c[:, 1, :].unsqueeze(1).broadcast_to([128, 4, 128])
        v4 = lambda ap: ap.rearrange("p (a b) -> p a b", a=4)
        fw.tt("dve", (t1, v4(t1[:])), (bq, v4(bq[:, :])), (c, cosb), ALU.mult)
        fw.tt("dve", (t2, v4(t2[:])), (bqb, v4(bqb[:, :])), (c, sinb), ALU.mult)
        qt = qtb[i % 2]
        fw.tt("pool", (qt, qt[:]), (t1, v4(t1[:])), (t2, v4(t2[:])), ALU.add)
        fw.dma("act", qT_s, qT_s[:, :, i * 128:(i + 1) * 128], qt, qt[:], out_part=i)
        fw.tt("dve", (t1, t1[:, 0:128]), (bk, bk[:, 0:128], 0), (c, c[:, 0, :]), ALU.mult)
        fw.tt("dve", (t2, t2[:, 0:128]), (bk, bk[:, 128:256], 1), (c, c[:, 1, :]), ALU.mult)
        fw.tt("pool", (kT, kT[:, i * 128:(i + 1) * 128], i), (t1, t1[:, 0:128]), (t2, t2[:, 0:128]), ALU.add)
        fw.copy("act", (vA, vA[:, i, 0:128], i), (bv, bv[:, 0:128], 0))

    NT_B = NT if stop >= 2 else 0
    load_weight_bf16(fw, wsb, wB_d, NB)
    szb = [fw.sbuf([128, 512], F32, "szb") for _ in range(2)]
    xbb = [fw.sbuf([128, 6, 128], F32, "xbb") for _ in range(2)]
    dtt = fw.sbuf([128, 16], F32, "dtt")
    xbc_v = xbc_s[:, :].rearrange("(g p) t -> p g t", p=128)
    for i in range(NT_B):
        hT = hTb[i % 2]
        htm.make(xin_d, row_ap(i), Am, Bm, 0 if i < NTC else 1, hT, hT[:])
        bz, bx1, bx2, bd = banks[2], banks[3], banks[4], banks[5]
        for kc in range(KC):
            fw.mm(bz, bz[:, :], hT, hT[:, kc, :], wsb, wsb[:, kc, 0:512], start=(kc == 0), stop=(kc == KC - 1),
                  rhs_part=kc // 8)
        for grp in range(6):
            bank, col = (bx1, grp) if grp < 4 else (bx2, grp - 4)
            for kc in range(KC):
                fw.mm(bank, bank[:, col * 128:(col + 1) * 128], wsb, wsb[:, kc, 512 + grp * 128:512 + (grp + 1) * 128],
                      hT, hT[:, kc, :], start=(kc == 0), stop=(kc == KC - 1), out_part=col, lhsT_part=kc // 8)
        for kc in range(KC):
            fw.mm(bd, bd[:, 0:16], hT, hT[:, kc, :], wsb, wsb[:, kc, 1280:1296], start=(kc == 0), stop=(kc == KC - 1),
                  out_part=0, rhs_part=kc // 8)
        sz = szb[i % 2]
        xb = xbb[i % 2]
        fw.activation((sz, sz[:]), (bz, bz[:, :]), AF.Silu)
        fw.dma("sp", sz_s, sz_s[i * 128:(i + 1) * 128, :], sz, sz[:], out_part=i)
        fw.copy("dve", (xb, xb[:, 0:4, :]), (bx1, bx1[:, :].rearrange("p (a b) -> p a b", a=4)))
        fw.copy("dve", (xb, xb[:, 4:6, :]), (bx2, bx2[:, 0:256].rearrange("p (a b) -> p a b", a=2)))
        fw.dma("act", xbc_s, xbc_v[:, :, i * 128:(i + 1) * 128], xb, xb[:], out_part=i)
        fw.tt("dve", (dtt, dtt[:]), (bd, bd[:, 0:16], 0), (sm, sm[:, 0:16]), ALU.add)
        fw.activation((dtt, dtt[:]), (dtt, dtt[:]), AF.Exp)
        fw.activation((dtA, dtA[:, i, :], i), (dtt, dtt[:]), AF.Ln, bias=1.0)
        fw.tt("dve", (aA, aA[:, i, :], i), (dtA, dtA[:, i, :], i), (sm, sm[:, 16:32]), ALU.mult)

    if debug:
        for nm, b, shp, dt_ in (("d_kT", kT, [128, T], BF16), ("d_vA", vA, [128, NT, 132], BF16),
                                ("d_dtA", dtA, [128, NT, 16], F32), ("d_aA", aA, [128, NT, 16], F32)):
            dd = fw.dram(nm, shp, dt_, "ExternalOutput")
            fw.dma("sp", dd, dd[:], b, b[:])
        dq = fw.dram("d_qT", [128, 4, T], BF16, "ExternalOutput")
        fw.dma("sp", dq, dq[:], qT_s, qT_s[:])
        dx = fw.dram("d_xbc", [768, T], F32, "ExternalOutput")
        fw.dma("sp", dx, dx[:], xbc_s, xbc_s[:])
        dz = fw.dram("d_sz", [T, 512], F32, "ExternalOutput")
        fw.dma("sp", dz, dz[:], sz_s, sz_s[:])
    ph1.__exit__(None, None, None)
    ph2 = fw.phase()
    ph2.__enter__()
    qin = [fw.sbuf([128, 4, 128], BF16, "qin") for _ in range(2)]
    pTb = [fw.sbuf([128, 512], BF16, "pT") for _ in range(6)]
    ob = [fw.sbuf([128, 4, 128], F32, "ob") for _ in range(2)]
    den = fw.sbuf([128, 4], F32, "den")
    npt = 0
    scale = 128 ** -0.5
    for i in range(NT if stop >= 3 else 0):
        if i < NTC:
            if not do_ctx_q:
                continue
            keys = [(kt, None) for kt in range(NTC)]
        else:
            j = i - NTC
            keys = [(kt, None) for kt in range(NTC)]
            if j - 1 >= 0:
                keys.append((i - 1, 0))
            keys.append((i, None))
            if j + 1 < NL:
                keys.append((i + 1, 1))
        bo = [banks[4], banks[5]]
        qi = qin[i % 2]
        fw.dma("act", qi, qi[:], qT_s, qT_s[:, :, i * 128:(i + 1) * 128], in_part=i)
        pts = []
        for n, (kt, m) in enumerate(keys):
            bs = banks[2 + (npt % 2)]
            pT = pTb[npt % 6]
            npt += 1
            fw.mm(bs, bs[:, :], kT, kT[:, kt * 128:(kt + 1) * 128], qi, qi[:], lhsT_part=kt)
            fw.activation((pT, pT[:]), (bs, bs[:, :]), AF.Exp, scale=scale)
            if m is not None:
                v4 = lambda ap: ap.rearrange("p (a b) -> p a b", a=4)
                fw.tt("dve", (pT, v4(pT[:])), (pT, v4(pT[:])),
                      (cstb, cstb[:, m, :].unsqueeze(1).broadcast_to([128, 4, 128])), ALU.mult)
            pts.append(pT)
        for h in range(4):
            b2 = bo[h // 2]
            o = (h % 2) * 132
            for n, (kt, m) in enumerate(keys):
                pT = pts[n]
                fw.mm(b2, b2[:, o:o + 129], pT, pT[:, h * 128:(h + 1) * 128], vA, vA[:, kt, 0:129],
                      start=(n == 0), stop=(n == len(keys) - 1), rhs_part=kt)
        o_sb = ob[i % 2]
        for h in range(4):
            b2 = bo[h // 2]
            o = (h % 2) * 132
            fw.tt("dve", (den, den[:, h:h + 1]), (b2, b2[:, o + 128:o + 129], h % 2), (sm, sm[:, 40 + h:41 + h]), ALU.add)
            fw.recip((den, den[:, h:h + 1]), (den, den[:, h:h + 1]))
            fw.ts("dve", (o_sb, o_sb[:, h, :]), (b2, b2[:, o:o + 128], h % 2), (den, den[:, h:h + 1]), ALU.mult)
        fw.dma("sp", attn_d, attn_d[i * 128:(i + 1) * 128, :], o_sb, o_sb[:].rearrange("p a b -> p (a b)"), out_part=i)

    ph2.__exit__(None, None, None)
    ur = [fw.sbuf([128, 6, 132], F32, "ur") for _ in range(2)]
    acc = fw.sbuf([128, 6, 128], F32, "acc")
    u = fw.sbuf([128, 6, 128], F32, "u")
    ubc = fw.sbuf([128, 2, 128], BF16, "ubc")
    xs_t = fw.sbuf([128, 512], F32, "xs_t")
    btok = fw.sbuf([128, 128], BF16, "btok")
    atri = fw.sbuf([128, 8, 128], F32, "atri")
    seg = fw.sbuf([128, 8, 128], F32, "seg")
    Mb = fw.sbuf([128, 8, 128], BF16, "Mb")
    cbT = fw.sbuf([128, 128], F32, "cbT")
    sml = fw.sbuf([128, 64], F32, "sml")
    xdt = fw.sbuf([128, 512], BF16, "xdt")
    xdtw = fw.sbuf([128, 512], BF16, "xdtw")
    hst = fw.sbuf([128, 512], F32, "hst")
    hbf = fw.sbuf([128, 512], BF16, "hbf")
    ydir = fw.sbuf([128, 512], F32, "ydir")
    tmp = fw.sbuf([128, 512], F32, "tmp")
    yin = [fw.sbuf([128, 512], F32, "yin") for _ in range(2)]
    szin = [fw.sbuf([128, 512], F32, "szin") for _ in range(2)]
    yout = [fw.sbuf([128, 512], F32, "yout") for _ in range(2)]
    bSt = fw.psum([128, 512], F32, "bankSt")
    bX, bM, bC0, bC1, bY, bYo = banks[0], banks[1], banks[2], banks[3], banks[4], banks[5]
    v8 = lambda ap: ap.rearrange("p (a b) -> p a b", a=8)
    nchunk = 0
    for d in range(2 if stop >= 4 else 0):
        tri_i = 1 if d == 0 else 2
        nm_i = 3 if d == 0 else 4
        last = 127 if d == 0 else 0
        order = list(range(NT)) if d == 0 else (list(range(NTC - 1, -1, -1)) + list(range(NT - 1, NTC - 1, -1)))
        fw.memset("dve", (hst, hst[:]), 0.0)
        fw.memset("dve", (hbf, hbf[:]), 0.0)
        for ci in order:
            s0, s1 = (0, LC) if ci < NTC else (LC, T)
            c0 = ci * 128
            lo, hi = max(c0 - 2, s0), min(c0 + 130, s1)
            urb = ur[nchunk % 2]
            if lo > c0 - 2 or hi < c0 + 130:
                fw.memset("pool", (urb, urb[:]), 0.0)
            fw.dma("sp", urb, urb[:, :, lo - (c0 - 2):hi - (c0 - 2)], xbc_s, xbc_v[:, :, lo:hi])
            for g in range(6):
                fw.ts("dve", (acc, acc[:, g, :], g), (urb, urb[:, g, 0:128]), (cw, cw[:, g, 0:1]), ALU.mult)
                for k in range(1, 5):
                    fw.stt((acc, acc[:, g, :], g), (urb, urb[:, g, k:k + 128]), (cw, cw[:, g, k:k + 1]),
                           (acc, acc[:, g, :], g), ALU.mult, ALU.add)
                fw.activation((u, u[:, g, :], g), (acc, acc[:, g, :], g), AF.Silu, bias=(cb, cb[:, g:g + 1]))
            fw.copy("pool", (ubc, ubc[:]), (u, u[:, 4:6, :]))
            for g in range(4):
                fw.tr(bX, bX[:, g * 128:(g + 1) * 128], u, u[:, g, :], idb, out_part=g, in_part=g)
            fw.tr(bM, bM[:, 256:384], u, u[:, 4, :], idb, out_part=2, in_part=4)
            fw.copy("act", (xs_t, xs_t[:]), (bX, bX[:, :]))
            fw.copy("act", (btok, btok[:]), (bM, bM[:, 256:384], 2))
            a_c = aA[:, ci, d * 8:(d + 1) * 8]
            dt_c = dtA[:, ci, d * 8:(d + 1) * 8]
            fw.tt("pool", (atri, atri[:]), (cst, cst[:, tri_i, :].unsqueeze(1).broadcast_to([128, 8, 128])),
                  (aA, a_c.unsqueeze(2).broadcast_to([128, 8, 128]), ci), ALU.mult)
            af = atri[:].rearrange("p a b -> p (a b)")
            fw.mm(bC0, bC0[:, :], cst, cst[:, 5, :], atri, af[:, 0:512])
            fw.mm(bC1, bC1[:, :], cst, cst[:, 5, :], atri, af[:, 512:1024])
            fw.mm(bM, bM[:, 128:136], cst, cst[:, tri_i, :], aA, a_c, out_part=1, rhs_part=ci)
            fw.mm(bM, bM[:, 0:128], ubc, ubc[:, 0, :], ubc, ubc[:, 1, :], out_part=0)
            fw.ts("dve", (sml, sml[:, 0:8]), (bM, bM[:, 128:136], 1), -1.0, ALU.mult)
            fw.activation((sml, sml[:, 8:16]), (bM, bM[:, 128:136], 1), AF.Exp)
            fw.copy("act", (cbT, cbT[:]), (bM, bM[:, 0:128], 0))
            for e in range(8):
                bc = bC0 if e < 4 else bC1
                o = (e % 4) * 128
                fw.stt((seg, seg[:, e, :], e), (bc, bc[:, o:o + 128]), (sml, sml[:, e:e + 1]),
                       (cst, cst[:, nm_i, :]), ALU.add, ALU.add)
            fw.activation((sml, sml[:, 16:20]), (bC0, v4b(bC0)[:, :, last]), AF.Exp)
            fw.activation((sml, sml[:, 20:24]), (bC1, v4b(bC1)[:, :, last]), AF.Exp)
            fw.activation((seg, seg[:]), (seg, seg[:]), AF.Exp)
            fw.tt("dve", (Mb, Mb[:]), (seg, seg[:]), (cbT, cbT[:].unsqueeze(1).broadcast_to([128, 8, 128])), ALU.mult)
            fw.tt("dve", (sml, sml[:, 24:32]), (dtA, dt_c, ci), (seg, seg[:, :, last]), ALU.mult)
            fw.tt("dve", (xdt, v8(xdt[:])), (xs_t, v8(xs_t[:])), (dtA, dt_c.unsqueeze(2).broadcast_to([128, 8, 64]), ci), ALU.mult)
            fw.tt("dve", (xdtw, v8(xdtw[:])), (xs_t, v8(xs_t[:])), (sml, sml[:, 24:32].unsqueeze(2).broadcast_to([128, 8, 64])), ALU.mult)
            for e in range(8):
                fw.mm(bY, bY[:, e * 64:(e + 1) * 64], Mb, Mb[:, e, :], xdt, xdt[:, e * 64:(e + 1) * 64], out_part=e)
            fw.mm(bYo, bYo[:, :], ubc, ubc[:, 1, :], hbf, hbf[:])
            fw.mm(bSt, bSt[:, :], btok, btok[:], xdtw, xdtw[:])
            fw.tt("dve", (tmp, v8(tmp[:])), (bYo, v8(bYo[:, :])), (sml, sml[:, 8:16].unsqueeze(2).broadcast_to([128, 8, 64])), ALU.mult)
            fw.tt("dve", (ydir, ydir[:]), (tmp, tmp[:]), (bY, bY[:, :]), ALU.add)
            fw.tt("dve", (hst, v8(hst[:])), (hst, v8(hst[:])), (sml, sml[:, 16:24].unsqueeze(2).broadcast_to([128, 8, 64])), ALU.mult)
            fw.tt("dve", (hst, hst[:]), (hst, hst[:]), (bSt, bSt[:, :]), ALU.add)
            fw.copy("pool", (hbf, hbf[:]), (hst, hst[:]))
            yo = yout[nchunk % 2]
            if d == 0:
                fw.tt("pool", (tmp, v8(tmp[:])), (xs_t, v8(xs_t[:])), (sm, sm[:, 32:40].unsqueeze(2).broadcast_to([128, 8, 64])), ALU.mult)
                fw.tt("pool", (yo, yo[:]), (tmp, tmp[:]), (ydir, ydir[:]), ALU.add)
                fw.dma("sp", ya_s, ya_s[c0:c0 + 128, :], yo, yo[:], out_part=ci)
            else:
                yi, zi = yin[nchunk % 2], szin[nchunk % 2]
                fw.dma("act", yi, yi[:], ya_s, ya_s[c0:c0 + 128, :], in_part=ci)
                fw.dma("act", zi, zi[:], sz_s, sz_s[c0:c0 + 128, :], in_part=ci)
                fw.tt("pool", (yi, yi[:]), (yi, yi[:]), (ydir, ydir[:]), ALU.add)
                fw.tt("pool", (yo, yo[:]), (yi, yi[:]), (zi, zi[:]), ALU.mult)
                fw.dma("sp", yg_d, yg_d[c0:c0 + 128, :], yo, yo[:], out_part=ci)
            nchunk += 1
    return fw.finish()


def v4b(bank):
    return bank[:, :].rearrange("p (a b) -> p a b", a=4)


def _pk(v):
    return np.ascontiguousarray(np.asarray(v, np.float32).reshape(-1, 128).T)


def _bc(v, n=128):
    return np.broadcast_to(np.asarray(v, np.float32)[None, :], (n, len(v)))


def mixer_consts():
    k = np.arange(128)
    triL = (k[:, None] <= k[None, :]).astype(np.float32)
    triU = (k[:, None] >= k[None, :]).astype(np.float32)
    cst = np.stack([np.eye(128, dtype=np.float32), triL, triU, (triL - 1) * 30000.0, (triU - 1) * 30000.0,
                    np.ones((128, 128), np.float32)], axis=1)
    return np.ascontiguousarray(cst)


def rope_tables(S, LC, grid_w=64, theta=10000.0):
    half = 64
    freqs = theta ** (-np.arange(0, half, 2, dtype=np.float32) / half)
    t = np.arange(S)
    row, col = (t // grid_w).astype(np.float32), (t % grid_w).astype(np.float32)
    d = np.arange(128)
    f = freqs[d % 32]
    pos = np.where((d < 64)[:, None], row[None, :], col[None, :])
    ang = (pos * f[:, None]).astype(np.float32)
    cos = np.cos(ang).astype(np.float32)
    sgn = np.where((d % 64) < 32, -1.0, 1.0).astype(np.float32)
    sin = (np.sin(ang) * sgn[:, None]).astype(np.float32)
    T = LC + S
    cosT = np.ones((128, T), np.float32)
    sinT = np.zeros((128, T), np.float32)
    cosT[:, LC:] = cos
    sinT[:, LC:] = sin
    return cosT, sinT


def mixer_weight_cols(g):
    q = np.arange(g * 512, (g + 1) * 512)
    perm = np.arange(128).reshape(4, 32)[[1, 0, 3, 2]].reshape(-1)
    qb = (q.reshape(4, 128)[:, perm]).reshape(-1)
    kk = 4096 + g * 128 + np.arange(128)
    kb = kk[perm]
    vv = 4096 + 512 + g * 128 + np.arange(128)
    z = 2048 + g * 512 + np.arange(512)
    xs = 5120 + g * 512 + np.arange(512)
    Bc = 5120 + 2048 + g * 128 + np.arange(128)
    Cc = 5120 + 2560 + g * 128 + np.arange(128)
    dt = np.concatenate([8192 + g * 8 + np.arange(8), 8192 + 32 + g * 8 + np.arange(8)])
    return np.concatenate([q, qb, kk, kb, vv]), np.concatenate([z, xs, Bc, Cc, dt])


def mixer_in_map(g, xin, norm_pre, sc_ctx, sh_ctx, sc_lat, sh_lat, w_in, conv_w, conv_b, dt_bias, a_log, d_skip,
                 attn_sink, cst, cosT, sinT):
    ca, cbb = mixer_weight_cols(g)
    cc = np.concatenate([g * 512 + np.arange(512), 2048 + g * 128 + np.arange(128), 2560 + g * 128 + np.arange(128)])
    small = np.zeros((128, 64), np.float32)
    hs = slice(g * 8, (g + 1) * 8)
    small[:, 0:8] = dt_bias[0, hs]
    small[:, 8:16] = dt_bias[1, hs]
    small[:, 16:24] = a_log[0, hs]
    small[:, 24:32] = a_log[1, hs]
    small[:, 32:40] = d_skip[hs]
    small[:, 40:44] = attn_sink[g * 4:(g + 1) * 4]
    return {
        "xin": xin,
        "gT": np.ascontiguousarray(np.stack([_pk(norm_pre), _pk(norm_pre)], axis=1)),
        "scT": np.ascontiguousarray(np.stack([_pk(sc_ctx), _pk(sc_lat)], axis=1)),
        "shT": np.ascontiguousarray(np.stack([_pk(sh_ctx), _pk(sh_lat)], axis=1)),
        "wA": np.ascontiguousarray(w_in[:, ca]),
        "wB": np.ascontiguousarray(w_in[:, cbb]),
        "cosT": cosT, "sinT": sinT,
        "convw": np.ascontiguousarray(conv_w[:, cc].T.reshape(6, 128, 5).transpose(1, 0, 2)),
        "convb": np.ascontiguousarray(conv_b[cc].reshape(6, 128).T),
        "small": small,
        "cst": cst,
    }


def build_post(sets, D=4096):
    fw = FW()
    NTl = len(sets)
    R = NTl * 128
    nset = max(sets) + 1
    KC = D // 128
    mix_d = fw.dram("mix", [R, D], F32, "ExternalInput")
    x_d = fw.dram("xres", [R, D], F32, "ExternalInput")
    w_d = fw.dram("wout", [D, D], F32, "ExternalInput")
    nrm_d = fw.dram("nrmT", [128, 1, KC], F32, "ExternalInput")
    gp_d = fw.dram("gpost", [nset, 128, D], F32, "ExternalInput")
    np_d = fw.dram("npost", [128, D], F32, "ExternalInput")
    id_d = fw.dram("ident", [128, 128], F32, "ExternalInput")
    out_d = fw.dram("x1", [R, D], F32, "ExternalOutput")
    y_s = fw.dram("y_s", [R, D], F32, "Internal")
    ident = fw.sbuf([128, 128], F32, "ident")
    fw.dma("sp", ident, ident[:], id_d, id_d[:])
    An = fw.sbuf([128, 1, KC], F32, "An")
    Bn = fw.sbuf([128, 1, KC], F32, "Bn")
    fw.dma("sp", An, An[:], nrm_d, nrm_d[:])
    fw.memset("dve", (Bn, Bn[:]), 0.0)
    catT = fw.sbuf([128, KC, R], BF16, "catT")
    banks = [fw.psum([128, 512], F32, "bank") for _ in range(4)]
    with fw.phase():
        htm = HTMaker(fw, D, ident, None, segs=[(0, D // 2), (D // 2, D)])
        for t in range(NTl):
            htm.make(mix_d, mix_d[t * 128:(t + 1) * 128, :], An, Bn, 0, catT, catT[:, :, t * 128:(t + 1) * 128])
    with fw.phase():
        wb = [fw.sbuf([128, KC, 512], BF16, "wb") for _ in range(2)]
        yb = [fw.sbuf([128, 512], F32, "yb") for _ in range(2)]
        n = 0
        for dc in range(D // 512):
            w = wb[dc % 2]
            src = w_d[:, dc * 512:(dc + 1) * 512].rearrange("(kc p) n -> p kc n", p=128)
            for k0 in range(0, KC, 8):
                fw.dma("pool", w, w[:, k0:k0 + 8, :], w_d, src[:, k0:k0 + 8, :], out_part=k0 // 8)
            for t in range(NTl):
                bk = banks[n % 4]
                y = yb[n % 2]
                n += 1
                for kc in range(KC):
                    fw.mm(bk, bk[:, :], catT, catT[:, kc, t * 128:(t + 1) * 128], w, w[:, kc, :], start=(kc == 0),
                          stop=(kc == KC - 1), rhs_part=kc // 8)
                fw.copy("act", (y, y[:]), (bk, bk[:, :]))
                fw.dma("sp", y_s, y_s[t * 128:(t + 1) * 128, dc * 512:(dc + 1) * 512], y, y[:], out_part=(t, dc))
    with fw.phase():
        gp = [fw.sbuf([128, D], F32, "gp") for _ in range(nset)]
        npb = fw.sbuf([128, D], F32, "npb")
        fw.dma("sp", npb, npb[:], np_d, np_d[:])
        for i in range(nset):
            fw.dma("act", gp[i], gp[i][:], gp_d, gp_d[i])
            fw.tt("pool", (gp[i], gp[i][:]), (gp[i], gp[i][:]), (npb, npb[:]), ALU.mult)
        yt = [fw.sbuf([128, D], F32, "yt") for _ in range(2)]
        xt = [fw.sbuf([128, D], F32, "xt") for _ in range(2)]
        junk = fw.sbuf([128, D], F32, "junk")
        ss = [fw.sbuf([128, 2], F32, "ss") for _ in range(2)]
        for t in range(NTl):
            y, x, s_ = yt[t % 2], xt[t % 2], ss[t % 2]
            fw.dma("sp", y, y[:], y_s, y_s[t * 128:(t + 1) * 128, :])
            fw.dma("act", x, x[:], x_d, x_d[t * 128:(t + 1) * 128, :])
            fw.activation((junk, junk[:]), (y, y[:]), AF.Square)
            fw.rsum("dve", (s_, s_[:, 0:1]), (junk, junk[:]))
            fw.activation((s_, s_[:, 1:2]), (s_, s_[:, 0:1]), AF.Sqrt, scale=1.0 / D, bias=1e-6)
            fw.recip((s_, s_[:, 1:2]), (s_, s_[:, 1:2]))
            fw.stt((y, y[:]), (y, y[:]), (s_, s_[:, 1:2]), (gp[sets[t]], gp[sets[t]][:]), ALU.mult, ALU.mult)
            fw.tt("pool", (x, x[:]), (x, x[:]), (y, y[:]), ALU.add)
            fw.dma("sp", out_d, out_d[t * 128:(t + 1) * 128, :], x, x[:], out_part=t)
    return fw.finish()


def build_ffn(sets, H, moe, D=4096, TB=4):
    fw = FW()
    NTl = len(sets)
    assert NTl % TB == 0
    R = NTl * 128
    nset = max(sets) + 1
    KC = D // 128
    HW = H * 128
    x_d = fw.dram("x1", [R, D], F32, "ExternalInput")
    g_d = fw.dram("gT", [128, nset, KC], F32, "ExternalInput")
    sc_d = fw.dram("scT", [128, nset, KC], F32, "ExternalInput")
    sh_d = fw.dram("shT", [128, nset, KC], F32, "ExternalInput")
    wg_d = fw.dram("wg", [D, HW], F32, "ExternalInput")
    wu_d = fw.dram("wu", [D, HW], F32, "ExternalInput")
    wd_d = fw.dram("wd", [HW, D], F32, "ExternalInput")
    id_d = fw.dram("ident", [128, 128], F32, "ExternalInput")
    out_d = fw.dram("fpart", [R, D], F32, "ExternalOutput")
    ident = fw.sbuf([128, 128], F32, "ident")
    fw.dma("sp", ident, ident[:], id_d, id_d[:])
    gT = fw.sbuf([128, nset, KC], F32, "gT")
    Am = fw.sbuf([128, nset, KC], F32, "Am")
    Bm = fw.sbuf([128, nset, KC], F32, "Bm")
    fw.dma("sp", gT, gT[:], g_d, g_d[:])
    fw.dma("sp", Am, Am[:], sc_d, sc_d[:])
    fw.dma("sp", Bm, Bm[:], sh_d, sh_d[:])
    fw.stt((Am, Am[:]), (Am, Am[:]), 1.0, (gT, gT[:]), ALU.add, ALU.mult)
    banks = [fw.psum([128, 512], F32, "bank") for _ in range(6)]
    if moe:
        rt_d = fw.dram("router", [D, 8], F32, "ExternalInput")
        es_d = fw.dram("esel", [128, 8], F32, "ExternalInput")
        rt = fw.sbuf([128, KC, 8], BF16, "rt")
        fw.dma("pool", rt, rt[:], rt_d, rt_d[:, :].rearrange("(kc p) n -> p kc n", p=128))
        esel = fw.sbuf([128, 8], F32, "esel")
        fw.dma("sp", esel, esel[:], es_d, es_d[:])
        lg = fw.sbuf([128, 8], F32, "lg")
        lg2 = fw.sbuf([128, 8], F32, "lg2")
        eq1 = fw.sbuf([128, 8], F32, "eq1")
        eq2 = fw.sbuf([128, 8], F32, "eq2")
        sm = fw.sbuf([128, 8], F32, "smm")
        cbc = fw.sbuf([128, 128], F32, "cbc")
        combT = fw.sbuf([128, TB * 128], F32, "combT")
    htm = HTMaker(fw, D, ident, None)
    hT = fw.sbuf([128, KC, TB * 128], BF16, "hTblk")
    aT = fw.sbuf([128, H, TB * 128], BF16, "aTblk")
    wgb = [fw.sbuf([128, KC, 128], BF16, "wgb") for _ in range(2)]
    wub = [fw.sbuf([128, KC, 128], BF16, "wub") for _ in range(2)]
    wdb = fw.sbuf([128, H, 512], BF16, "wdb")
    tmp = [fw.sbuf([128, TB * 128], F32, "tmp") for _ in range(2)]
    ob = [fw.sbuf([128, 512], F32, "ob") for _ in range(2)]
    NW = TB * 128
    nj = 0
    no = 0
    for blk in range(NTl // TB):
        for t in range(TB):
            ti = blk * TB + t
            htm.make(x_d, x_d[ti * 128:(ti + 1) * 128, :], Am, Bm, sets[ti], hT, hT[:, :, t * 128:(t + 1) * 128])
        if moe:
            for t in range(TB):
                bl = banks[4]
                for kc in range(KC):
                    fw.mm(bl, bl[:, 0:8], hT, hT[:, kc, t * 128:(t + 1) * 128], rt, rt[:, kc, :], start=(kc == 0),
                          stop=(kc == KC - 1))
                fw.copy("dve", (lg, lg[:]), (bl, bl[:, 0:8]))
                fw.op("dve", lambda e: e.reduce_max(out=sm[:, 0:1], in_=lg[:], axis=AX.X), reads=[lg], writes=[sm])
                fw.ts("dve", (eq1, eq1[:]), (lg, lg[:]), (sm, sm[:, 0:1]), ALU.is_equal)
                fw.stt((lg2, lg2[:]), (eq1, eq1[:]), -1e30, (lg, lg[:]), ALU.mult, ALU.add)
                fw.op("dve", lambda e: e.reduce_max(out=sm[:, 1:2], in_=lg2[:], axis=AX.X), reads=[lg2, sm], writes=[sm])
                fw.ts("dve", (eq2, eq2[:]), (lg2, lg2[:]), (sm, sm[:, 1:2]), ALU.is_equal)
                fw.tt("dve", (sm, sm[:, 2:3]), (sm, sm[:, 1:2]), (sm, sm[:, 0:1]), ALU.subtract)
                fw.activation((sm, sm[:, 3:4]), (sm, sm[:, 2:3]), AF.Exp)
                fw.ts("dve", (sm, sm[:, 3:4]), (sm, sm[:, 3:4]), 1.0, ALU.add)
                fw.recip((sm, sm[:, 4:5]), (sm, sm[:, 3:4]))
                fw.ts("dve", (sm, sm[:, 5:6]), (sm, sm[:, 4:5]), -1.0, ALU.mult, 1.0, ALU.add)
                fw.ts("dve", (eq1, eq1[:]), (eq1, eq1[:]), (sm, sm[:, 4:5]), ALU.mult)
                fw.stt((eq1, eq1[:]), (eq2, eq2[:]), (sm, sm[:, 5:6]), (eq1, eq1[:]), ALU.mult, ALU.add)
                fw.tt("dve", (eq1, eq1[:]), (eq1, eq1[:]), (esel, esel[:]), ALU.mult)
                fw.rsum("dve", (sm, sm[:, 6:7]), (eq1, eq1[:]))
                fw.copy("dve", (cbc, cbc[:]), (sm, sm[:, 6:7].broadcast_to([128, 128])))
                bc = banks[5]
                fw.mm(bc, bc[:, 0:128], cbc, cbc[:], ident, ident[:])
                fw.copy("act", (combT, combT[:, t * 128:(t + 1) * 128], t), (bc, bc[:, 0:128]))
        for j in range(H):
            wg, wu = wgb[nj % 2], wub[nj % 2]
            nj += 1
            fw.dma("pool", wg, wg[:], wg_d, wg_d[:, j * 128:(j + 1) * 128].rearrange("(kc p) n -> p kc n", p=128))
            fw.dma("pool", wu, wu[:], wu_d, wu_d[:, j * 128:(j + 1) * 128].rearrange("(kc p) n -> p kc n", p=128))
            bg, bu = banks[(nj % 2) * 2], banks[(nj % 2) * 2 + 1]
            for kc in range(KC):
                fw.mm(bg, bg[:, 0:NW], wg, wg[:, kc, :], hT, hT[:, kc, :], start=(kc == 0), stop=(kc == KC - 1))
            for kc in range(KC):
                fw.mm(bu, bu[:, 0:NW], wu, wu[:, kc, :], hT, hT[:, kc, :], start=(kc == 0), stop=(kc == KC - 1))
            tm = tmp[nj % 2]
            fw.activation((tm, tm[:]), (bg, bg[:, 0:NW]), AF.Silu)
            if moe:
                fw.tt("pool", (tm, tm[:]), (tm, tm[:]), (combT, combT[:]), ALU.mult)
            fw.tt("dve", (aT, aT[:, j, :], j), (tm, tm[:]), (bu, bu[:, 0:NW]), ALU.mult)
        for dc in range(D // 512):
            fw.dma("pool", wdb, wdb[:], wd_d, wd_d[:, dc * 512:(dc + 1) * 512].rearrange("(h p) n -> p h n", p=128))
            for t in range(TB):
                ti = blk * TB + t
                bk = banks[4 + (no % 2)]
                o = ob[no % 2]
                no += 1
                for j in range(H):
                    fw.mm(bk, bk[:, :], aT, aT[:, j, t * 128:(t + 1) * 128], wdb, wdb[:, j, :], start=(j == 0),
                          stop=(j == H - 1), lhsT_part=j)
                fw.copy("act", (o, o[:]), (bk, bk[:, :]))
                fw.dma("sp", out_d, out_d[ti * 128:(ti + 1) * 128, dc * 512:(dc + 1) * 512], o, o[:], out_part=(ti, dc))
    return fw.finish()


def build_combine(sets, D=4096, NP=8):
    fw = FW()
    NTl = len(sets)
    R = NTl * 128
    nset = max(sets) + 1
    p_d = fw.dram("parts", [NP, R, D], F32, "ExternalInput")
    x_d = fw.dram("x1", [R, D], F32, "ExternalInput")
    gp_d = fw.dram("gpost", [nset, 128, D], F32, "ExternalInput")
    np_d = fw.dram("npost", [128, D], F32, "ExternalInput")
    out_d = fw.dram("x2", [R, D], F32, "ExternalOutput")
    gp = [fw.sbuf([128, D], F32, "gp") for _ in range(nset)]
    npb = fw.sbuf([128, D], F32, "npb")
    fw.dma("sp", npb, npb[:], np_d, np_d[:])
    for i in range(nset):
        fw.dma("act", gp[i], gp[i][:], gp_d, gp_d[i])
        fw.tt("pool", (gp[i], gp[i][:]), (gp[i], gp[i][:]), (npb, npb[:]), ALU.mult)
    acc = [fw.sbuf([128, D], F32, "acc") for _ in range(2)]
    pin = [fw.sbuf([128, D], F32, "pin") for _ in range(3)]
    xt = [fw.sbuf([128, D], F32, "xt") for _ in range(2)]
    junk = fw.sbuf([128, D], F32, "junk")
    ss = [fw.sbuf([128, 2], F32, "ss") for _ in range(2)]
    npin = 0
    for t in range(NTl):
        a, x, s_ = acc[t % 2], xt[t % 2], ss[t % 2]
        rows = slice(t * 128, (t + 1) * 128)
        fw.dma("sp", a, a[:], p_d, p_d[0, rows, :])
        fw.dma("act", x, x[:], x_d, x_d[rows, :])
        for k in range(1, NP):
            p = pin[npin % 3]
            npin += 1
            fw.dma("sp" if k % 2 else "act", p, p[:], p_d, p_d[k, rows, :])
            fw.tt("dve" if k % 2 else "pool", (a, a[:]), (a, a[:]), (p, p[:]), ALU.add)
        fw.activation((junk, junk[:]), (a, a[:]), AF.Square)
        fw.rsum("dve", (s_, s_[:, 0:1]), (junk, junk[:]))
        fw.activation((s_, s_[:, 1:2]), (s_, s_[:, 0:1]), AF.Sqrt, scale=1.0 / D, bias=1e-6)
        fw.recip((s_, s_[:, 1:2]), (s_, s_[:, 1:2]))
        fw.stt((a, a[:]), (a, a[:]), (s_, s_[:, 1:2]), (gp[sets[t]], gp[sets[t]][:]), ALU.mult, ALU.mult)
        fw.tt("pool", (x, x[:]), (x, x[:]), (a, a[:]), ALU.add)
        fw.dma("sp", out_d, out_d[rows, :], x, x[:], out_part=t)
    return fw.finish()


_CACHE = {}


def _get(key, fn):
    if key not in _CACHE:
        _CACHE[key] = fn()
    return _CACHE[key]


def _run(nc, maps):
    return run_bass_kernel_spmd(nc, maps, core_ids=list(range(len(maps)))).results


def _pad_rows(a, n):
    out = np.zeros((n,) + a.shape[1:], a.dtype)
    out[:a.shape[0]] = a
    return out


def kernel(x, c, ctx, c_ctx, ada_w, ada_b, norm_mix_pre, norm_mix_post, norm_ffn_pre, norm_ffn_post,
           w_in, attn_sink, attn_norm, conv_w, conv_b, dt_bias, a_log, d_skip, ssm_norm, w_out,
           ffn_w_gate, ffn_w_up, ffn_w_down, moe_router, moe_w_gate, moe_w_up, moe_w_down):
    f32 = np.float32
    A = lambda a: np.asarray(a, f32)
    x_cur, xc_cur = A(x), A(ctx)
    Bsz, S, D = x_cur.shape
    LC = xc_cur.shape[1]
    T = LC + S
    depth = w_in.shape[0]
    mod = run_mod(A(c), A(c_ctx), A(ada_w), A(ada_b))
    ch = lambda i, r, k: mod[i, r, k * D:(k + 1) * D]
    cst = mixer_consts()
    cosT, sinT = rope_tables(S, LC)
    ident = np.eye(128, dtype=f32)
    QL, QC = S // 4, LC // 4
    for i in range(depth):
        last = i == depth - 1
        nc1 = _get(("mixer", S, LC), lambda: build_mixer(S, LC))
        maps = []
        for b in range(Bsz):
            xin = np.concatenate([xc_cur[b], x_cur[b]], axis=0)
            for g in range(4):
                maps.append(mixer_in_map(g, xin, A(norm_mix_pre[i]), ch(i, 2, 1), ch(i, 2, 0), ch(i, b, 1), ch(i, b, 0),
                                         A(w_in[i]), A(conv_w[i]), A(conv_b[i]), A(dt_bias[i]), A(a_log[i]),
                                         A(d_skip[i]), A(attn_sink[i]), cst, cosT, sinT))
        res = _run(nc1, maps)
        del maps
        mix = np.zeros((Bsz, T, D), f32)
        for b in range(Bsz):
            for g in range(4):
                r = res[b * 4 + g]
                mix[b, :, g * 512:(g + 1) * 512] = r["attn"]
                mix[b, :, D // 2 + g * 512:D // 2 + (g + 1) * 512] = r["yg"]
        del res
        sets2 = [0] * (QL // 128) + ([] if last else [1])
        nc2 = _get(("post", tuple(sets2)), lambda: build_post(sets2))
        nrmT = np.ascontiguousarray(_pk(np.concatenate([A(attn_norm[i]), A(ssm_norm[i])]))[:, None, :])
        maps = []
        for b in range(Bsz):
            for qd in range(4):
                mrows = [mix[b, LC + qd * QL:LC + (qd + 1) * QL]]
                xrows = [x_cur[b, qd * QL:(qd + 1) * QL]]
                gps = [_bc(ch(i, b, 2))]
                if not last:
                    mrows.append(_pad_rows(mix[b, qd * QC:(qd + 1) * QC], 128))
                    xrows.append(_pad_rows(xc_cur[b, qd * QC:(qd + 1) * QC], 128))
                    gps.append(_bc(ch(i, 2, 2)))
                maps.append({"mix": np.concatenate(mrows, 0), "xres": np.concatenate(xrows, 0), "wout": A(w_out[i]),
                             "nrmT": nrmT, "gpost": np.ascontiguousarray(np.stack(gps)),
                             "npost": np.ascontiguousarray(_bc(A(norm_mix_post[i]))), "ident": ident})
        res = _run(nc2, maps)
        del maps, mix
        x1 = np.zeros_like(x_cur)
        xc1 = np.zeros_like(xc_cur)
        for b in range(Bsz):
            for qd in range(4):
                r = res[b * 4 + qd]["x1"]
                x1[b, qd * QL:(qd + 1) * QL] = r[:QL]
                if not last:
                    xc1[b, qd * QC:(qd + 1) * QC] = r[QL:QL + QC]
        del res
        if last:
            rows3 = np.concatenate([x1[b] for b in range(Bsz)], 0)
            sets3 = sum([[b] * (S // 128) for b in range(Bsz)], [])
            setrows = list(range(Bsz))
        else:
            rows3 = np.concatenate([xc1[b] for b in range(Bsz)] + [x1[b] for b in range(Bsz)], 0)
            sets3 = [Bsz] * (Bsz * LC // 128) + sum([[b] * (S // 128) for b in range(Bsz)], [])
            setrows = list(range(Bsz)) + [2]
        gT = np.ascontiguousarray(np.stack([_pk(A(norm_ffn_pre[i]))] * len(setrows), 1))
        scT = np.ascontiguousarray(np.stack([_pk(ch(i, r, 4)) for r in setrows], 1))
        shT = np.ascontiguousarray(np.stack([_pk(ch(i, r, 3)) for r in setrows], 1))
        j = i // 2
        moe = (i % 2 == 1)
        maps = []
        if not moe:
            dff = ffn_w_gate.shape[2]
            per = dff // 8
            H = (per + 127) // 128
            for k in range(8):
                sl = slice(k * per, (k + 1) * per)
                wg = np.zeros((D, H * 128), f32); wg[:, :per] = ffn_w_gate[j][:, sl]
                wu = np.zeros((D, H * 128), f32); wu[:, :per] = ffn_w_up[j][:, sl]
                wd = np.zeros((H * 128, D), f32); wd[:per] = ffn_w_down[j][sl]
                maps.append({"x1": rows3, "gT": gT, "scT": scT, "shT": shT, "wg": wg, "wu": wu, "wd": wd, "ident": ident})
        else:
            H = moe_w_gate.shape[3] // 128
            for k in range(8):
                esel = np.zeros((128, 8), f32); esel[:, k] = 1.0
                maps.append({"x1": rows3, "gT": gT, "scT": scT, "shT": shT, "wg": A(moe_w_gate[j, k]), "wu": A(moe_w_up[j, k]),
                             "wd": A(moe_w_down[j, k]), "ident": ident, "router": A(moe_router[j]), "esel": esel})
        nc3 = _get(("ffn", tuple(sets3), H, moe), lambda: build_ffn(sets3, H, moe))
        res = _run(nc3, maps)
        del maps, rows3
        fparts = [r["fpart"] for r in res]
        del res
        nc4 = _get(("comb", tuple(sets2)), lambda: build_combine(sets2))
        coff = 0 if last else Bsz * LC
        maps = []
        for b in range(Bsz):
            for qd in range(4):
                lo = coff + b * S + qd * QL
                prow = [np.stack([fp[lo:lo + QL] for fp in fparts])]
                xrows = [x1[b, qd * QL:(qd + 1) * QL]]
                gps = [_bc(ch(i, b, 5))]
                if not last:
                    cl = b * LC + qd * QC
                    pc = np.zeros((8, 128, D), f32)
                    pc[:, :QC] = np.stack([fp[cl:cl + QC] for fp in fparts])
                    prow.append(pc)
                    xrows.append(_pad_rows(xc1[b, qd * QC:(qd + 1) * QC], 128))
                    gps.append(_bc(ch(i, 2, 5)))
                maps.append({"parts": np.concatenate(prow, 1), "x1": np.concatenate(xrows, 0),
                             "gpost": np.ascontiguousarray(np.stack(gps)),
                             "npost": np.ascontiguousarray(_bc(A(norm_ffn_post[i])))})
        res = _run(nc4, maps)
        del maps, fparts
        x_new = np.zeros_like(x_cur)
        xc_new = np.zeros_like(xc_cur)
        for b in range(Bsz):
            for qd in range(4):
                r = res[b * 4 + qd]["x2"]
                x_new[b, qd * QL:(qd + 1) * QL] = r[:QL]
                if not last:
                    xc_new[b, qd * QC:(qd + 1) * QC] = r[QL:QL + QC]
        x_cur, xc_cur = x_new, xc_new
    return x_cur.astype(f32)
```
